# Optimizing a Trainium2 kernel written in Bass

```python
import math
import jax, jax.numpy as jnp
from jax import lax
import numpy as np

D_MODEL = 2048
BATCH = 4
SEQ = 4096
DEPTH = 2

MIX_WIDTH = D_MODEL
DIFF_V_DIM = 128
DIFF_QK_DIM = 64
N_DIFF_HEADS = (MIX_WIDTH // 2) // DIFF_V_DIM
SB_HEAD_DIM = 128
N_SB_HEADS = (MIX_WIDTH // 2) // SB_HEAD_DIM
DIFF_QK_COLS = N_DIFF_HEADS * 2 * DIFF_QK_DIM
DIFF_V_COLS = N_DIFF_HEADS * DIFF_V_DIM
SB_COLS = N_SB_HEADS * SB_HEAD_DIM
IN_COLS = 2 * DIFF_QK_COLS + DIFF_V_COLS + 3 * SB_COLS
Q_BLOCK = 128
NUM_BUCKETS = 32
MAX_DISTANCE = 128
N_GROUPS = 4
EXPERTS_PER_GROUP = 8
N_EXPERTS = N_GROUPS * EXPERTS_PER_GROUP
TOP_K = 2
D_EXPERT = D_MODEL // 2
MOE_BLOCK = 128
LN_EPS = 1e-5
SUBLN_EPS = 1e-5
ADA_SCALE = 0.1
DEEPNORM_ALPHA = (2 * DEPTH) ** 0.25
DEEPNORM_BETA = (8 * DEPTH) ** -0.25

kernel_name = "hymba_diff_stickbreak_hmoe_deepnorm"


def layer_norm(h, g, b):
    hf = h.astype(jnp.float32)
    mu = jnp.mean(hf, axis=-1, keepdims=True)
    var = jnp.mean(jnp.square(hf - mu), axis=-1, keepdims=True)
    out = (hf - mu) * lax.rsqrt(var + LN_EPS) * g.astype(jnp.float32) + b.astype(jnp.float32)
    return out.astype(h.dtype)


def t5_bucket(rel):
    n = jnp.maximum(rel, 0)
    max_exact = NUM_BUCKETS // 2
    large = max_exact + (jnp.log(jnp.maximum(n, 1).astype(jnp.float32) / max_exact)
                         / math.log(MAX_DISTANCE / max_exact) * (NUM_BUCKETS - max_exact)).astype(jnp.int32)
    large = jnp.minimum(large, NUM_BUCKETS - 1)
    return jnp.where(n < max_exact, n, large)


def diff_attention(q, k, v, bias_table, lam, lam_init, subln_g):
    B, S, H, _, DQK = q.shape
    nqb = S // Q_BLOCK
    scale = DQK ** -0.5
    kpos = jnp.arange(S)
    qb = q.reshape(B, nqb, Q_BLOCK, H, 2, DQK).transpose(1, 0, 2, 3, 4, 5)

    def block(args):
        qi, bi = args
        qpos = bi * Q_BLOCK + jnp.arange(Q_BLOCK)
        logits = jnp.einsum('bqhmd,bkhmd->bmhqk', qi, k).astype(jnp.float32) * scale
        bias = bias_table[t5_bucket(qpos[:, None] - kpos[None, :])]
        logits = logits + jnp.transpose(bias, (3, 2, 0, 1)).astype(jnp.float32)[None]
        causal = kpos[None, :] <= qpos[:, None]
        p = jax.nn.softmax(jnp.where(causal, logits, -jnp.inf), axis=-1)
        a = p[:, 0] - lam * p[:, 1]
        return jnp.einsum('bhqk,bkhd->bqhd', a.astype(v.dtype), v)

    o = lax.map(block, (qb, jnp.arange(nqb)))
    o = o.transpose(1, 0, 2, 3, 4).reshape(B, S, H, -1)
    of = o.astype(jnp.float32)
    of = of * lax.rsqrt(jnp.mean(jnp.square(of), axis=-1, keepdims=True) + SUBLN_EPS)
    of = of * subln_g.astype(jnp.float32) * (1.0 - lam_init)
    return of.astype(v.dtype).reshape(B, S, -1)


def stick_breaking_attention(q, k, v):
    B, S, H, DH = q.shape
    nqb = S // Q_BLOCK
    scale = DH ** -0.5
    kpos = jnp.arange(S)
    qb = q.reshape(B, nqb, Q_BLOCK, H, DH).transpose(1, 0, 2, 3, 4)

    def block(args):
        qi, bi = args
        qpos = bi * Q_BLOCK + jnp.arange(Q_BLOCK)
        z = jnp.einsum('bqhd,bkhd->bhqk', qi, k).astype(jnp.float32) * scale
        strict = kpos[None, :] < qpos[:, None]
        log_keep = jnp.where(strict, jax.nn.log_sigmoid(-z), 0.0)
        suffix = lax.cumsum(log_keep, axis=3, reverse=True) - log_keep
        a = jnp.where(strict, jnp.exp(jax.nn.log_sigmoid(z) + suffix), 0.0)
        return jnp.einsum('bhqk,bkhd->bqhd', a.astype(v.dtype), v)

    o = lax.map(block, (qb, jnp.arange(nqb)))
    return o.transpose(1, 0, 2, 3, 4).reshape(B, S, H * DH)


def hierarchical_moe(u, w_group, b_group, w_router, b_router, w_gate, w_up, w_down):
    B, S, D = u.shape
    M = B * S
    xt = u.reshape(M, D)
    gprob = jax.nn.softmax((xt @ w_group + b_group).astype(jnp.float32), axis=-1)
    gp, gidx = lax.top_k(gprob, 1)
    elog = (xt @ w_router + b_router).astype(jnp.float32).reshape(M, N_GROUPS, EXPERTS_PER_GROUP)
    sel = jnp.take_along_axis(elog, gidx[:, :, None], axis=1)[:, 0]
    ev, eidx = lax.top_k(sel, TOP_K)
    ew = jax.nn.softmax(ev, axis=-1) * gp
    eid = gidx * EXPERTS_PER_GROUP + eidx

    n_assign = M * TOP_K
    e_flat = eid.reshape(-1).astype(jnp.int32)
    w_flat = ew.reshape(-1)
    tok_flat = jnp.repeat(jnp.arange(M, dtype=jnp.int32), TOP_K)
    order = jnp.argsort(e_flat)
    sorted_e = e_flat[order]
    counts = jnp.bincount(e_flat, length=N_EXPERTS)
    starts = jnp.cumsum(counts) - counts
    padded = ((counts + MOE_BLOCK - 1) // MOE_BLOCK) * MOE_BLOCK
    pends = jnp.cumsum(padded)
    pstarts = pends - padded
    dest = pstarts[sorted_e] + (jnp.arange(n_assign, dtype=jnp.int32) - starts[sorted_e])
    n_blocks = (n_assign + N_EXPERTS * (MOE_BLOCK - 1) + MOE_BLOCK - 1) // MOE_BLOCK
    P = n_blocks * MOE_BLOCK
    pad_tok = jnp.zeros((P,), jnp.int32).at[dest].set(tok_flat[order])
    pad_w = jnp.zeros((P,), jnp.float32).at[dest].set(w_flat[order])
    block_e = jnp.minimum(jnp.searchsorted(pends, jnp.arange(n_blocks, dtype=jnp.int32) * MOE_BLOCK,
                                           side='right'), N_EXPERTS - 1)
    xg = xt[pad_tok].reshape(n_blocks, MOE_BLOCK, D)

    def expert_block(args):
        xb, e = args
        h = jax.nn.silu(xb @ w_gate[e]) * (xb @ w_up[e])
        return h @ w_down[e]

    yb = lax.map(expert_block, (xg, block_e)).reshape(P, D)
    y = jnp.zeros((M, D), u.dtype).at[pad_tok].add(yb * pad_w[:, None].astype(u.dtype))
    return y.reshape(B, S, D)


def setup_inputs(seed: int = 0) -> dict:
    key = jax.random.key(seed)
    ks = jax.random.split(key, 18)
    D = D_MODEL
    nrm = jax.random.normal
    beta = DEEPNORM_BETA
    col_scale = jnp.concatenate([
        jnp.ones((2 * DIFF_QK_COLS,)), jnp.full((DIFF_V_COLS,), beta),
        jnp.ones((2 * SB_COLS,)), jnp.full((SB_COLS,), beta)])
    return {
        "x": nrm(ks[0], (BATCH, SEQ, D), jnp.float32),
        "c": nrm(ks[1], (BATCH, D), jnp.float32),
        "w_ada": nrm(ks[2], (DEPTH, D, 6 * D), jnp.float32) * (D ** -0.5) * ADA_SCALE,
        "b_ada": 0.02 * nrm(ks[3], (DEPTH, 6 * D), jnp.float32),
        "w_in": nrm(ks[4], (DEPTH, D, IN_COLS), jnp.float32) * (D ** -0.5) * col_scale,
        "diff_lambda": 0.1 * nrm(ks[5], (DEPTH, 4, DIFF_QK_DIM), jnp.float32),
        "diff_subln_g": 1.0 + 0.02 * nrm(ks[6], (DEPTH, DIFF_V_DIM), jnp.float32),
        "rel_bias": 0.2 * nrm(ks[7], (NUM_BUCKETS, N_DIFF_HEADS, 2), jnp.float32),
        "w_o": nrm(ks[8], (DEPTH, MIX_WIDTH, D), jnp.float32) * (MIX_WIDTH ** -0.5) * beta,
        "ln_g": 1.0 + 0.02 * nrm(ks[9], (DEPTH, 2, D), jnp.float32),
        "ln_b": 0.02 * nrm(ks[10], (DEPTH, 2, D), jnp.float32),
        "w_group": nrm(ks[11], (DEPTH, D, N_GROUPS), jnp.float32) * (D ** -0.5),
        "b_group": 0.01 * nrm(ks[12], (DEPTH, N_GROUPS), jnp.float32),
        "w_router": nrm(ks[13], (DEPTH, D, N_EXPERTS), jnp.float32) * (D ** -0.5),
        "b_router": 0.01 * nrm(ks[14], (DEPTH, N_EXPERTS), jnp.float32),
        "w_gate": nrm(ks[15], (DEPTH, N_EXPERTS, D, D_EXPERT), jnp.float32) * (D ** -0.5),
        "w_up": nrm(ks[16], (DEPTH, N_EXPERTS, D, D_EXPERT), jnp.float32) * (D ** -0.5),
        "w_down": nrm(ks[17], (DEPTH, N_EXPERTS, D_EXPERT, D), jnp.float32) * (D_EXPERT ** -0.5) * beta,
    }


def reference(x, c, w_ada, b_ada, w_in, diff_lambda, diff_subln_g, rel_bias, w_o, ln_g, ln_b,
              w_group, b_group, w_router, b_router, w_gate, w_up, w_down):
    B, S, D = x.shape
    splits = [DIFF_QK_COLS, 2 * DIFF_QK_COLS, 2 * DIFF_QK_COLS + DIFF_V_COLS,
              2 * DIFF_QK_COLS + DIFF_V_COLS + SB_COLS, 2 * DIFF_QK_COLS + DIFF_V_COLS + 2 * SB_COLS]
    for l in range(DEPTH):
        mod = jax.nn.silu(c) @ w_ada[l] + b_ada[l]
        sh1, sc1, g1, sh2, sc2, g2 = jnp.split(mod[:, None, :], 6, axis=-1)

        u = x * (1.0 + sc1) + sh1
        proj = u @ w_in[l]
        dq, dk, dv, sq, sk, sv = jnp.split(proj, splits, axis=-1)
        lam_init = 0.8 - 0.6 * math.exp(-0.3 * l)
        lam_p = diff_lambda[l].astype(jnp.float32)
        lam = jnp.exp(jnp.sum(lam_p[0] * lam_p[1])) - jnp.exp(jnp.sum(lam_p[2] * lam_p[3])) + lam_init
        a_out = diff_attention(
            dq.reshape(B, S, N_DIFF_HEADS, 2, DIFF_QK_DIM),
            dk.reshape(B, S, N_DIFF_HEADS, 2, DIFF_QK_DIM),
            dv.reshape(B, S, N_DIFF_HEADS, DIFF_V_DIM),
            rel_bias, lam, lam_init, diff_subln_g[l])
        b_out = stick_breaking_attention(
            sq.reshape(B, S, N_SB_HEADS, SB_HEAD_DIM),
            sk.reshape(B, S, N_SB_HEADS, SB_HEAD_DIM),
            sv.reshape(B, S, N_SB_HEADS, SB_HEAD_DIM))
        mix = jnp.concatenate([a_out, b_out], axis=-1) @ w_o[l]
        x = layer_norm(DEEPNORM_ALPHA * x + (1.0 + g1) * mix, ln_g[l, 0], ln_b[l, 0])

        u2 = x * (1.0 + sc2) + sh2
        y = hierarchical_moe(u2, w_group[l], b_group[l], w_router[l], b_router[l],
                             w_gate[l], w_up[l], w_down[l])
        x = layer_norm(DEEPNORM_ALPHA * x + (1.0 + g2) * y, ln_g[l, 1], ln_b[l, 1])
    return x
```

```python
import math
import numpy as np
from contextlib import ExitStack
import concourse.bass as bass
import concourse.mybir as mybir
from concourse.bass_utils import run_bass_kernel_spmd

F32 = mybir.dt.float32
BF16 = mybir.dt.bfloat16
I32 = mybir.dt.int32
AF = mybir.ActivationFunctionType
ALU = mybir.AluOpType
AX = mybir.AxisListType

NEG = -30000.0
LN_EPS = 1e-5
SUBLN_EPS = 1e-5


class H:
    __slots__ = ("w", "r", "joined")

    def __init__(self):
        self.w = {}
        self.r = {}
        self.joined = False


class Sched:
    ENGS = ("pe", "act", "dve", "pool", "sp")

    def __init__(self, nc, stack, n_dma_sems=8):
        self.nc = nc
        self.eng = {"pe": nc.tensor, "act": nc.scalar, "dve": nc.vector, "pool": nc.gpsimd, "sp": nc.sync}
        self.sem, self.tick = {}, {}
        self.known = {e: {} for e in self.ENGS}
        for e in self.ENGS:
            self.sem[e] = stack.enter_context(nc.semaphore("s_" + e))
            self.tick[e] = 0
        self.dsem, self.dcnt = {}, {}
        for q in ("sp", "pool", "act"):
            self.dsem[q] = [stack.enter_context(nc.semaphore("d_%s%d" % (q, i))) for i in range(n_dma_sems)]
            self.dcnt[q] = 0
        self.semobj = {}
        for e in self.ENGS:
            self.semobj[("e", e)] = self.sem[e]
        for q in self.dsem:
            for i, sm in enumerate(self.dsem[q]):
                self.semobj[("d", q, i)] = sm
        self.pend_r = {e: [] for e in self.ENGS}
        self.pend_w = {e: [] for e in self.ENGS}
        self.all_dma_ev = {}
        self.n_wait = 0
        self.n_ins = 0

    def need(self, e, key, val):
        if self.known[e].get(key, 0) >= val:
            return
        self.eng[e].wait_ge(self.semobj[key], val)
        self.known[e][key] = val
        self.n_wait += 1

    def _deps(self, e, reads, writes, skip_self, join):
        me = ("e", e)
        for h in reads:
            for k, v in h.w.items():
                if not (skip_self and k == me):
                    self.need(e, k, v)
        for h in writes:
            if join and h.joined:
                continue
            for k, v in h.w.items():
                if not (skip_self and k == me):
                    self.need(e, k, v)
            for k, v in h.r.items():
                if not (skip_self and k == me):
                    self.need(e, k, v)

    def _commit(self, ev, reads, writes, join):
        k, v = ev
        for h in reads:
            if h.r.get(k, 0) < v:
                h.r[k] = v
            h.joined = False
        for h in writes:
            if join and h.joined:
                h.w[k] = v
            else:
                h.w = {k: v}
                h.r = {}
                h.joined = join

    def op(self, e, fn, reads=(), writes=(), inc=True, skip_self=False):
        self._deps(e, reads, writes, skip_self, False)
        ins = fn(self.eng[e])
        self.n_ins += 1
        if inc:
            self.tick[e] += 1
            ins.then_inc(self.sem[e], 1)
            ev = (("e", e), self.tick[e])
            self._commit(ev, list(reads) + self.pend_r[e], list(writes) + self.pend_w[e], False)
            self.pend_r[e] = []
            self.pend_w[e] = []
            return ev
        self.pend_r[e] += list(reads)
        self.pend_w[e] += list(writes)
        return None

    def dma(self, q, out, in_, reads=(), writes=(), indirect=None, join=True, **kw):
        self._deps(q, reads, writes, False, join)
        pool = self.dsem[q]
        K = len(pool)
        c = self.dcnt[q]
        i = c % K
        key = ("d", q, i)
        if c >= K:
            self.need(q, key, 16 * (c // K))
        if indirect is None:
            ins = self.eng[q].dma_start(out=out, in_=in_, **kw)
        else:
            ins = indirect(self.eng[q])
        ins.then_inc(pool[i], 16)
        self.dcnt[q] = c + 1
        self.n_ins += 1
        ev = (key, 16 * (c // K + 1))
        self._commit(ev, reads, writes, join)
        self.all_dma_ev[key] = ev
        return ev

    def barrier(self, engines=None):
        engines = engines or self.ENGS
        evs = [(("e", w), self.tick[w]) for w in self.ENGS if self.tick[w] > 0]
        evs += list(self.all_dma_ev.values())
        for e in engines:
            for (k, v) in evs:
                if k == ("e", e):
                    continue
                self.need(e, k, v)


class Ring:
    def __init__(self, items):
        self.items = items
        self.i = 0

    def next(self):
        it = self.items[self.i % len(self.items)]
        self.i += 1
        return it


class Cfg:
    def __init__(self, D=2048, S=4096, E=32, G=4, CAP=256, depth=2):
        self.D, self.S, self.E, self.G, self.CAP, self.depth = D, S, E, G, CAP, depth
        self.EPG = E // G
        self.F = D // 2
        self.DC = D // 128
        self.FC = self.F // 128
        self.NB = S // 128
        self.NOWN = self.NB // 2
        self.TO = S // 2
        self.HD = (D // 2) // 128
        self.HS = (D // 2) // 128
        self.NH = self.HD + self.HS
        self.HG = min(4, self.HD)
        self.TBW = min(512, self.TO)
        self.alpha = (2 * depth) ** 0.25


class Prog:
    def __init__(self, cfg, layers, debug=False):
        self.cfg = cfg
        self.layers = layers
        self.debug = debug
        self.nc = bass.Bass("TRN2", target_bir_lowering=False)
        self.t = {}
        self._nmc = 0

    def bounds_reg(self):
        if getattr(self, "_breg", None) is None:
            self._breg = self.nc.gpsimd.to_reg(self.cfg.E * self.cfg.CAP - 1)
        return self._breg

    def nm(self, name):
        self._nmc += 1
        return "%s_u%d" % (name, self._nmc)

    def din(self, name, shape, dt=F32):
        self.t[name] = self.nc.dram_tensor(name, list(shape), dt, kind="ExternalInput").ap()

    def dscr(self, name, shape, dt, out=False):
        kind = "ExternalOutput" if (out or self.debug) else "Internal"
        self.t[name] = self.nc.dram_tensor(name, list(shape), dt, kind=kind).ap()

    def declare(self):
        c = self.cfg
        L = len(self.layers)
        D, S, TO, E, F = c.D, c.S, c.TO, c.E, c.F
        self.din("x_full", [S, D])
        self.din("x_own", [TO, D])
        self.din("c_lay", [128, c.DC])
        self.din("w_ada", [L, D, 6 * D])
        self.din("b_ada", [L, 6 * D])
        self.din("w_in", [L, D, 3 * D])
        self.din("dlam", [L, 256])
        self.din("subg", [L, 128])
        self.din("w_o", [L, D, D])
        self.din("ln_g", [L, 2 * D])
        self.din("ln_b", [L, 2 * D])
        self.din("w_gr", [L, D, 4 + E])
        self.din("b_gr", [L, 4 + E])
        self.din("w_gate", [L, E, D, F])
        self.din("w_up", [L, E, D, F])
        self.din("w_down", [L, E, F, D])
        self.din("nearb", [c.HD, 128, 3 * 2 * 128])
        self.din("farb", [128, c.HD * 2])
        self.din("sbm01", [128, 3 * 128])
        self.din("sbneg", [128, 3 * 128])
        self.dscr("out", [TO, D], F32, out=True)
        self.dscr("uT", [c.DC, 128, S], BF16)
        self.dscr("uTo", [c.DC, 128, TO], BF16)
        self.dscr("qT", [c.NH, 128, TO], BF16)
        self.dscr("kT", [c.NH, 128, S], BF16)
        self.dscr("vall", [S, c.NH * 128], BF16)
        self.dscr("attnT", [c.NH, 128, TO], BF16)
        self.dscr("x1", [TO, D], F32)
        self.dscr("xg", [E * c.CAP, D], BF16)
        self.dscr("yg", [E * c.CAP, D], F32)
        self.dscr("modd", [1, 6 * D], F32)
        self.dscr("xnext", [TO, D], F32)
        if self.debug:
            self.dscr("dbg_modT", [128, 6 * c.DC], F32)
            self.dscr("dbg_route", [TO, 8], F32)

    def build(self):
        nc, c = self.nc, self.cfg
        self.declare()
        with ExitStack() as st:
            self.s = Sched(nc, st)
            s = self.s
            sb = lambda name, shape, dt: st.enter_context(nc.sbuf_tensor(self.nm(name), shape, dt))
            self.ident = sb("ident", [128, 128], F32); self.h_ident = H()
            self.identb = sb("identb", [128, 128], BF16); self.h_identb = H()
            self.modT = sb("modT", [128, 6 * c.DC], F32); self.h_modT = H()
            self.one1 = sb("one1", [1, 128], F32); self.h_one1 = H()
            NTO = c.TO // 128
            self.slots = sb("slots", [128, NTO, 2], I32); self.h_slots = H()
            self.wts = sb("wts", [128, NTO, 2], F32); self.h_wts = H()
            s.op("pool", lambda e: e.memset(self.ident[:], 1.0), writes=[self.h_ident])
            s.op("pool", lambda e: e.affine_select(out=self.ident[:], in_=self.ident[:], pattern=[[-1, 128]],
                                                   compare_op=ALU.is_equal, fill=0.0, base=0, channel_multiplier=1),
                 reads=[self.h_ident], writes=[self.h_ident])
            s.op("pool", lambda e: e.tensor_copy(out=self.identb[:], in_=self.ident[:]), reads=[self.h_ident], writes=[self.h_identb])
            s.op("pool", lambda e: e.memset(self.one1[:], 1.0), writes=[self.h_one1])
            for li, lact in enumerate(self.layers):
                self.cur_layer = lact
                l = li
                self.phase_A(l)
                s.barrier()
                self.phase_B(l, li)
                s.barrier()
                self.phase_C(l)
                s.barrier()
                self.phase_D(l)
                s.barrier()
                self.phase_E(l, li)
                s.barrier()
                self.phase_F(l)
                s.barrier()
                self.phase_G(l, li, last=(li == len(self.layers) - 1))
                s.barrier()
            s.barrier(engines=("sp",))
        return nc

    def phase_A(self, l):
        nc, s, c, t = self.nc, self.s, self.cfg, self.t
        D, DC = c.D, c.DC
        with ExitStack() as st:
            sb = lambda name, shape, dt: st.enter_context(nc.sbuf_tensor(self.nm(name), shape, dt))
            clay = sb("clay", [128, DC], F32); h_clay = H()
            sT = sb("sT", [128, DC], F32); h_sT = H()
            wa = [sb("wa%d" % i, [128, DC, 512], F32) for i in range(2)]; h_wa = [H(), H()]
            modrow = sb("modrow", [1, 6 * D], F32); h_modrow = H()
            brow = sb("brow", [1, 6 * D], F32); h_brow = H()
            ps = [st.enter_context(nc.psum_tensor(self.nm("psA%d" % i), [128, 512], F32)) for i in range(2)]; h_ps = [H(), H()]
            psT = st.enter_context(nc.psum_tensor(self.nm("psAT"), [128, 512], F32)); h_psT = H()
            s.dma("sp", clay[:], t["c_lay"][:, :], writes=[h_clay])
            s.dma("sp", brow[:], t["b_ada"][l:l + 1, :], writes=[h_brow])
            s.op("act", lambda e: e.activation(out=sT[:], in_=clay[:], func=AF.Silu), reads=[h_clay], writes=[h_sT])
            nblk = 6 * D // 512
            for j in range(nblk):
                b = j % 2
                s.dma("sp", wa[b][:], t["w_ada"][l, :, j * 512:(j + 1) * 512].rearrange("(kc p) n -> p kc n", p=128), writes=[h_wa[b]])
                for kc in range(DC):
                    s.op("pe", lambda e: e.matmul(ps[b][0:1, :], lhsT=sT[:, kc:kc + 1], rhs=wa[b][:, kc, :], start=(kc == 0), stop=(kc == DC - 1)),
                         reads=[h_sT, h_wa[b]], writes=[h_ps[b]], inc=(kc == DC - 1), skip_self=True)
                s.op("dve", lambda e: e.tensor_tensor(out=modrow[0:1, j * 512:(j + 1) * 512], in0=ps[b][0:1, :], in1=brow[0:1, j * 512:(j + 1) * 512], op=ALU.add),
                     reads=[h_ps[b], h_brow], writes=[h_modrow])
            n = 6 * DC
            for idx in range(n):
                s.op("pe", lambda e: e.matmul(psT[:, idx:idx + 1], lhsT=modrow[0:1, idx * 128:(idx + 1) * 128], rhs=self.one1[0:1, 0:1], start=True, stop=True),
                     reads=[h_modrow, self.h_one1], writes=[h_psT], inc=(idx == n - 1), skip_self=True)
            s.op("dve", lambda e: e.tensor_copy(out=self.modT[:], in_=psT[:, 0:n]), reads=[h_psT], writes=[self.h_modT])
            for k in (1, 4):
                s.op("dve", lambda e: e.tensor_scalar(out=self.modT[:, k * DC:(k + 1) * DC], in0=self.modT[:, k * DC:(k + 1) * DC], scalar1=1.0, scalar2=None, op0=ALU.add),
                     reads=[self.h_modT], writes=[self.h_modT])
            s.dma("sp", t["modd"][:, :], modrow[:], reads=[h_modrow])
            if self.debug:
                s.dma("sp", t["dbg_modT"][:, :], self.modT[:], reads=[self.h_modT])

    def phase_B(self, l, li):
        nc, s, c, t = self.nc, self.s, self.cfg, self.t
        D, DC, S, TO, W = c.D, c.DC, c.S, c.TO, c.TBW
        NT = W // 128
        with ExitStack() as st:
            sb = lambda name, shape, dt: st.enter_context(nc.sbuf_tensor(self.nm(name), shape, dt))
            xin = [sb("xin%d" % i, [128, NT, D], F32) for i in range(2)]; h_xin = [H(), H()]
            uo = [sb("uo%d" % i, [128, DC, W], BF16) for i in range(2)]; h_uo = [H(), H()]
            banks = Ring([(st.enter_context(nc.psum_tensor(self.nm("psB%d" % i), [128, 512], F32)), H()) for i in range(4)])
            jobs = []
            for g in range(S // W):
                jobs.append(("full", g))
            for g in range(TO // W):
                jobs.append(("own", g))
            for n, (kind, g) in enumerate(jobs):
                b = n % 2
                if kind == "full":
                    src = self.src_full(li, g * W, W)
                    dst = t["uT"].rearrange("kc p s -> p kc s")[:, :, g * W:(g + 1) * W]
                else:
                    src = self.src_own(li, g * W, W)
                    dst = t["uTo"].rearrange("kc p s -> p kc s")[:, :, g * W:(g + 1) * W]
                for (tt0, ntt, ap) in src:
                    s.dma("sp", xin[b][:, tt0:tt0 + ntt, :], ap, writes=[h_xin[b]])
                for kc in range(DC):
                    bank, hb = banks.next()
                    for tt in range(NT):
                        s.op("pe", lambda e: e.transpose(bank[:, tt * 128:(tt + 1) * 128], xin[b][:, tt, kc * 128:(kc + 1) * 128], self.ident[:]),
                             reads=[h_xin[b], self.h_ident], writes=[hb], inc=(tt == NT - 1), skip_self=True)
                    s.op("act", lambda e: e.activation(out=uo[b][:, kc, :], in_=bank[:, 0:W], func=AF.Identity,
                                                       scale=self.modT[:, DC + kc:DC + kc + 1], bias=self.modT[:, kc:kc + 1]),
                         reads=[hb, self.h_modT], writes=[h_uo[b]])
                s.dma("sp", dst, uo[b][:], reads=[h_uo[b]])

    def src_full(self, li, r0, n):
        t = self.t
        if li == 0:
            return [(0, n // 128, t["x_full"][r0:r0 + n, :].rearrange("(t p) d -> p t d", p=128))]
        raise NotImplementedError

    def src_own(self, li, r0, n):
        t = self.t
        if li == 0:
            return [(0, n // 128, t["x_own"][r0:r0 + n, :].rearrange("(t p) d -> p t d", p=128))]
        return [(0, n // 128, t["xnext"][r0:r0 + n, :].rearrange("(t p) d -> p t d", p=128))]

    def phase_C(self, l):
        nc, s, c, t = self.nc, self.s, self.cfg, self.t
        D, DC, S, TO, W, HG = c.D, c.DC, c.S, c.TO, c.TBW, c.HG
        GW = HG * 128
        NT = W // 128
        with ExitStack() as st:
            sb = lambda name, shape, dt: st.enter_context(nc.sbuf_tensor(self.nm(name), shape, dt))
            wq = [sb("wq%d" % i, [128, DC, GW], BF16) for i in range(2)]
            wk = [sb("wk%d" % i, [128, DC, GW], BF16) for i in range(2)]
            wv = [sb("wv%d" % i, [128, DC, GW], BF16) for i in range(2)]
            h_w = [H(), H()]
            ub = [sb("ub%d" % i, [128, DC, W], BF16) for i in range(2)]; h_ub = [H(), H()]
            ko = Ring([(sb("ko%d" % i, [128, W], BF16), H()) for i in range(4)])
            vo = Ring([(sb("vo%d" % i, [128, GW], BF16), H()) for i in range(4)])
            banks = Ring([(st.enter_context(nc.psum_tensor(self.nm("psC%d" % i), [128, 512], F32)), H()) for i in range(6)])
            groups = []
            for g in range(c.HD // HG):
                groups.append((g * GW, D // 2 + g * GW, D + g * GW, g * HG))
            for g in range(c.HS // HG):
                groups.append((3 * D // 2 + g * GW, 2 * D + g * GW, 5 * D // 2 + g * GW, c.HD + g * HG))
            nub = 0
            nev = 0
            for gi, (q0, k0, v0, nh0) in enumerate(groups):
                wb = gi % 2
                for (wt, c0) in ((wq[wb], q0), (wk[wb], k0), (wv[wb], v0)):
                    s.dma("pool", wt[:], t["w_in"][l, :, c0:c0 + GW].rearrange("(kc p) n -> p kc n", p=128), writes=[h_w[wb]])
                for tb in range(S // W):
                    u = nub % 2; nub += 1
                    s.dma("sp", ub[u][:], t["uT"].rearrange("kc p s -> p kc s")[:, :, tb * W:(tb + 1) * W], writes=[h_ub[u]])
                    for hh in range(HG):
                        bank, hb = banks.next()
                        for kc in range(DC):
                            s.op("pe", lambda e: e.matmul(bank[:, 0:W], lhsT=wk[wb][:, kc, hh * 128:(hh + 1) * 128], rhs=ub[u][:, kc, :], start=(kc == 0), stop=(kc == DC - 1)),
                                 reads=[h_w[wb], h_ub[u]], writes=[hb], inc=(kc == DC - 1), skip_self=True)
                        o, ho = ko.next()
                        self.evac(nev, o[:], bank[:, 0:W], hb, ho); nev += 1
                        s.dma("sp", t["kT"][nh0 + hh, :, tb * W:(tb + 1) * W], o[:], reads=[ho])
                    for tt in range(NT):
                        bank, hb = banks.next()
                        for kc in range(DC):
                            s.op("pe", lambda e: e.matmul(bank[:, 0:GW], lhsT=ub[u][:, kc, tt * 128:(tt + 1) * 128], rhs=wv[wb][:, kc, :], start=(kc == 0), stop=(kc == DC - 1)),
                                 reads=[h_w[wb], h_ub[u]], writes=[hb], inc=(kc == DC - 1), skip_self=True)
                        o, ho = vo.next()
                        self.evac(nev, o[:], bank[:, 0:GW], hb, ho); nev += 1
                        r0 = tb * W + tt * 128
                        s.dma("sp", t["vall"][r0:r0 + 128, nh0 * 128:nh0 * 128 + GW], o[:], reads=[ho])
                for tb in range(TO // W):
                    u = nub % 2; nub += 1
                    s.dma("sp", ub[u][:], t["uTo"].rearrange("kc p s -> p kc s")[:, :, tb * W:(tb + 1) * W], writes=[h_ub[u]])
                    for hh in range(HG):
                        bank, hb = banks.next()
                        for kc in range(DC):
                            s.op("pe", lambda e: e.matmul(bank[:, 0:W], lhsT=wq[wb][:, kc, hh * 128:(hh + 1) * 128], rhs=ub[u][:, kc, :], start=(kc == 0), stop=(kc == DC - 1)),
                                 reads=[h_w[wb], h_ub[u]], writes=[hb], inc=(kc == DC - 1), skip_self=True)
                        o, ho = ko.next()
                        self.evac(nev, o[:], bank[:, 0:W], hb, ho); nev += 1
                        s.dma("sp", t["qT"][nh0 + hh, :, tb * W:(tb + 1) * W], o[:], reads=[ho])

    def evac(self, n, out, in_, h_in, h_out):
        s = self.s
        if n % 2 == 0:
            s.op("act", lambda e: e.copy(out=out, in_=in_), reads=[h_in], writes=[h_out])
        else:
            s.op("dve", lambda e: e.tensor_copy(out=out, in_=in_), reads=[h_in], writes=[h_out])

    def phase_D(self, l):
        nc, s, c, t = self.nc, self.s, self.cfg, self.t
        S, TO, NB, NOWN = c.S, c.TO, c.NB, c.NOWN
        lam_init = 0.8 - 0.6 * math.exp(-0.3 * self.cur_layer)
        with ExitStack() as st:
            sb = lambda name, shape, dt: st.enter_context(nc.sbuf_tensor(self.nm(name), shape, dt))
            A = type("A", (), {})()
            self.A = A
            A.kTs = [sb("kTs%d" % i, [128, S], BF16) for i in range(2)]
            A.qTs = [sb("qTs%d" % i, [128, TO], BF16) for i in range(2)]
            A.vaug = [sb("vaug%d" % i, [128, NB, 129], BF16) for i in range(2)]
            A.nb = [sb("nb%d" % i, [128, 3, 2, 128], F32) for i in range(2)]
            A.h_in = [H(), H()]
            A.attnTs = [sb("attnTs%d" % i, [128, TO], BF16) for i in range(2)]; A.h_attnTs = [H(), H()]
            A.farb = sb("farb", [128, c.HD * 2], F32); A.h_const = H()
            A.sbm01 = sb("sbm01", [128, 3, 128], F32)
            A.sbneg = sb("sbneg", [128, 3, 128], F32)
            A.triN = sb("triN", [128, 128], BF16)
            A.onescol = sb("onescol", [128, 1], BF16)
            A.trif = sb("trif", [128, 128], F32)
            A.neglam = sb("neglam", [128, 1], F32)
            A.gsub = sb("gsub", [128, 128], F32)
            A.et = Ring([(sb("et%d" % i, [128, 512], BF16), H()) for i in range(4)])
            A.tmpf = Ring([(sb("tmpf%d" % i, [128, 512], F32), H()) for i in range(2)])
            A.ebuf = Ring([(sb("ebuf%d" % i, [128, 512], F32), H()) for i in range(2)])
            A.spf = Ring([(sb("spf%d" % i, [128, 384], F32), H()) for i in range(2)])
            A.spb = Ring([(sb("spb%d" % i, [128, 512], BF16), H()) for i in range(2)])
            A.small = Ring([(sb("small%d" % i, [128, 16], F32), H()) for i in range(4)])
            A.o32 = Ring([(sb("o32_%d" % i, [128, 128], F32), H()) for i in range(2)])
            A.t32 = Ring([(sb("t32_%d" % i, [128, 128], F32), H()) for i in range(2)])
            A.junk = Ring([(sb("junk%d" % i, [128, 128], F32), H()) for i in range(2)])
            A.o16 = Ring([(sb("o16_%d" % i, [128, 128], BF16), H()) for i in range(2)])
            A.acc = Ring([(sb("acc%d" % i, [128, 128], F32), H()) for i in range(2)])
            A.carry = Ring([(sb("carry%d" % i, [128, 1], F32), H()) for i in range(2)])
            A.sbank = Ring([(st.enter_context(nc.psum_tensor(self.nm("psS%d" % i), [128, 512], F32)), H()) for i in range(3)])
            A.obank = [[(st.enter_context(nc.psum_tensor(self.nm("psO%d%d" % (i, m)), [128, 512], F32)), H()) for m in range(2)] for i in range(2)]
            A.tp = (st.enter_context(nc.psum_tensor(self.nm("psT"), [128, 1024], BF16)), H())
            dl = sb("dl", [1, 256], F32); h_dl = H()
            hc = A.h_const
            s.dma("sp", A.farb[:], t["farb"][:, :], writes=[hc])
            s.dma("sp", A.sbm01[:], t["sbm01"][:, :].rearrange("p (j q) -> p j q", j=3), writes=[hc])
            s.dma("sp", A.sbneg[:], t["sbneg"][:, :].rearrange("p (j q) -> p j q", j=3), writes=[hc])
            s.dma("sp", A.gsub[:], t["subg"][l:l + 1, :].to_broadcast([128, 128]), writes=[hc])
            s.dma("sp", dl[:], t["dlam"][l:l + 1, :], writes=[h_dl])
            triv = -11.3125
            A.sbscale2 = 1.0 / 11.3125
            s.op("pool", lambda e: e.memset(A.trif[:], triv), writes=[hc])
            s.op("pool", lambda e: e.affine_select(out=A.trif[:], in_=A.trif[:], pattern=[[-1, 128]], compare_op=ALU.is_ge,
                                                   fill=0.0, base=0, channel_multiplier=1), reads=[hc], writes=[hc])
            s.op("pool", lambda e: e.tensor_copy(out=A.triN[:], in_=A.trif[:]), reads=[hc], writes=[hc])
            s.op("pool", lambda e: e.memset(A.onescol[:], 1.0), writes=[hc])
            for i in range(2):
                s.op("pool", lambda e: e.memset(A.vaug[i][:, :, 128:129], 1.0), writes=[A.h_in[i]])
            s.op("dve", lambda e: e.tensor_scalar(out=A.gsub[:], in0=A.gsub[:], scalar1=float(1.0 - lam_init), scalar2=None, op0=ALU.mult), reads=[hc], writes=[hc])
            sm, hsm = A.small.next()
            s.op("dve", lambda e: e.tensor_tensor(out=dl[0:1, 0:64], in0=dl[0:1, 0:64], in1=dl[0:1, 64:128], op=ALU.mult), reads=[h_dl], writes=[h_dl])
            s.op("dve", lambda e: e.tensor_tensor(out=dl[0:1, 64:128], in0=dl[0:1, 128:192], in1=dl[0:1, 192:256], op=ALU.mult), reads=[h_dl], writes=[h_dl])
            s.op("dve", lambda e: e.tensor_reduce(out=sm[0:1, 0:2], in_=dl[0:1, 0:128].rearrange("p (a b) -> p a b", a=2), axis=AX.X, op=ALU.add), reads=[h_dl], writes=[hsm])
            s.op("act", lambda e: e.activation(out=sm[0:1, 2:4], in_=sm[0:1, 0:2], func=AF.Exp), reads=[hsm], writes=[hsm])
            s.op("dve", lambda e: e.tensor_tensor(out=sm[0:1, 4:5], in0=sm[0:1, 3:4], in1=sm[0:1, 2:3], op=ALU.subtract), reads=[hsm], writes=[hsm])
            s.op("dve", lambda e: e.tensor_scalar(out=sm[0:1, 5:6], in0=sm[0:1, 4:5], scalar1=float(-lam_init), scalar2=None, op0=ALU.add), reads=[hsm], writes=[hsm])
            bank, hb = A.sbank.next()
            s.op("pe", lambda e: e.matmul(bank[:, 0:1], lhsT=self.one1[0:1, 0:128], rhs=sm[0:1, 5:6], start=True, stop=True), reads=[hsm, self.h_one1], writes=[hb])
            s.op("dve", lambda e: e.tensor_copy(out=A.neglam[:], in_=bank[:, 0:1]), reads=[hb], writes=[hc])
            A.nhead = 0
            for h in range(c.HD):
                self.diff_head(l, h)
            for h in range(c.HS):
                self.sb_head(l, h)

    def load_head(self, nh):
        s, c, t, A = self.s, self.cfg, self.t, self.A
        k = A.nhead % 2
        A.nhead += 1
        hi = A.h_in[k]
        s.dma("sp", A.kTs[k][:], t["kT"][nh, :, :], writes=[hi])
        s.dma("sp", A.qTs[k][:], t["qT"][nh, :, :], writes=[hi])
        s.dma("sp", A.vaug[k][:, :, 0:128], t["vall"][:, nh * 128:(nh + 1) * 128].rearrange("(kb p) d -> p kb d", p=128), writes=[hi])
        if nh < c.HD:
            s.dma("sp", A.nb[k][:], t["nearb"][nh, :, :].rearrange("p (j m q) -> p j m q", j=3, m=2), writes=[hi])
        return k, hi

    def diff_head(self, l, h):
        nc, s, c, t, A = self.nc, self.s, self.cfg, self.t, self.A
        k, hi = self.load_head(h)
        kTs, qTs, vaug, nb = A.kTs[k], A.qTs[k], A.vaug[k], A.nb[k]
        attnTs, h_at = A.attnTs[k], A.h_attnTs[k]
        hc = A.h_const
        scale = 64 ** -0.5
        for i in range(c.NOWN):
            ob = A.obank[i % 2]
            nkb = 2 * i + 2
            qsl = slice(i * 128, (i + 1) * 128)
            for m in range(2):
                psl = slice(64 * m, 64 * m + 64)
                obk, hob = ob[m]
                nfar = max(0, 2 * i - 1)
                groups = []
                for g0 in range(0, nfar, 4):
                    groups.append(("far", list(range(g0, min(nfar, g0 + 4))), None))
                near = [(2 * i - 1 + j, j) for j in range(3) if 2 * i - 1 + j >= 0]
                groups.append(("near", [kb for kb, j in near], near[0][1]))
                done = 0
                for kind, kbs, j0 in groups:
                    n = len(kbs)
                    bank, hb = A.sbank.next()
                    for jj, kb in enumerate(kbs):
                        s.op("pe", lambda e: e.matmul(bank[:, jj * 128:(jj + 1) * 128], lhsT=kTs[psl, kb * 128:(kb + 1) * 128], rhs=qTs[psl, qsl], start=True, stop=True),
                             reads=[hi], writes=[hb], inc=(jj == n - 1), skip_self=True)
                    et, het = A.et.next()
                    if kind == "far":
                        s.op("act", lambda e: e.activation(out=et[:, 0:n * 128], in_=bank[:, 0:n * 128], func=AF.Exp, scale=scale, bias=A.farb[:, 2 * h + m:2 * h + m + 1]),
                             reads=[hb, hc], writes=[het])
                    else:
                        tf, htf = A.tmpf.next()
                        s.op("dve", lambda e: e.scalar_tensor_tensor(out=tf[:, 0:n * 128].rearrange("p (j q) -> p j q", j=n), in0=bank[:, 0:n * 128].rearrange("p (j q) -> p j q", j=n),
                                                                     scalar=scale, in1=nb[:, j0:j0 + n, m, :], op0=ALU.mult, op1=ALU.add),
                             reads=[hb, hi], writes=[htf])
                        s.op("act", lambda e: e.activation(out=et[:, 0:n * 128], in_=tf[:, 0:n * 128], func=AF.Exp), reads=[htf], writes=[het])
                    for jj, kb in enumerate(kbs):
                        s.op("pe", lambda e: e.matmul(obk[:, 0:129], lhsT=et[:, jj * 128:(jj + 1) * 128], rhs=vaug[:, kb, :], start=(done == 0), stop=(done == nkb - 1)),
                             reads=[het, hi], writes=[hob], inc=(jj == n - 1), skip_self=True)
                        done += 1
            (o0, ho0), (o1, ho1) = ob
            sm, hsm = A.small.next()
            s.op("dve", lambda e: e.reciprocal(out=sm[:, 0:1], in_=o0[:, 128:129]), reads=[ho0], writes=[hsm])
            s.op("dve", lambda e: e.reciprocal(out=sm[:, 1:2], in_=o1[:, 128:129]), reads=[ho1], writes=[hsm])
            s.op("dve", lambda e: e.tensor_tensor(out=sm[:, 2:3], in0=sm[:, 1:2], in1=A.neglam[:, 0:1], op=ALU.mult), reads=[hsm, hc], writes=[hsm])
            t32, ht32 = A.t32.next()
            s.op("dve", lambda e: e.tensor_scalar(out=t32[:], in0=o0[:, 0:128], scalar1=sm[:, 0:1], scalar2=None, op0=ALU.mult), reads=[ho0, hsm], writes=[ht32])
            o32, ho32 = A.o32.next()
            s.op("dve", lambda e: e.scalar_tensor_tensor(out=o32[:], in0=o1[:, 0:128], scalar=sm[:, 2:3], in1=t32[:], op0=ALU.mult, op1=ALU.add), reads=[ho1, hsm, ht32], writes=[ho32])
            self.head_out(i, o32, ho32, attnTs, h_at, subnorm=True)
        s.dma("sp", t["attnT"][h, :, :], attnTs[:], reads=[h_at])

    def head_out(self, i, o32, ho32, attnTs, h_at, subnorm):
        s, A = self.s, self.A
        hc = A.h_const
        o16, ho16 = A.o16.next()
        if subnorm:
            sm, hsm = A.small.next()
            jk, hjk = A.junk.next()
            s.op("dve", lambda e: e.tensor_tensor(out=jk[:], in0=o32[:], in1=o32[:], op=ALU.mult), reads=[ho32], writes=[hjk])
            s.op("dve", lambda e: e.tensor_reduce(out=sm[:, 0:1], in_=jk[:], axis=AX.X, op=ALU.add), reads=[hjk], writes=[hsm])
            s.op("dve", lambda e: e.tensor_scalar(out=sm[:, 1:2], in0=sm[:, 0:1], scalar1=1.0 / 128.0, scalar2=SUBLN_EPS, op0=ALU.mult, op1=ALU.add), reads=[hsm], writes=[hsm])
            s.op("act", lambda e: e.activation(out=sm[:, 2:3], in_=sm[:, 1:2], func=AF.Ln), reads=[hsm], writes=[hsm])
            s.op("act", lambda e: e.activation(out=sm[:, 3:4], in_=sm[:, 2:3], func=AF.Exp, scale=-0.5), reads=[hsm], writes=[hsm])
            s.op("dve", lambda e: e.scalar_tensor_tensor(out=o16[:], in0=o32[:], scalar=sm[:, 3:4], in1=A.gsub[:], op0=ALU.mult, op1=ALU.mult), reads=[ho32, hsm, hc], writes=[ho16])
        else:
            s.op("dve", lambda e: e.tensor_copy(out=o16[:], in_=o32[:]), reads=[ho32], writes=[ho16])
        tp, htp = A.tp
        s.op("pe", lambda e: e.transpose(tp[:, 0:128], o16[:], self.identb[:]), reads=[ho16, self.h_identb], writes=[htp])
        s.op("act", lambda e: e.copy(out=attnTs[:, i * 128:(i + 1) * 128], in_=tp[:, 0:128]), reads=[htp], writes=[h_at])

    def sb_head(self, l, h):
        nc, s, c, t, A = self.nc, self.s, self.cfg, self.t, self.A
        nh = c.HD + h
        k, hi = self.load_head(nh)
        kTs, qTs, vaug = A.kTs[k], A.qTs[k], A.vaug[k]
        attnTs, h_at = A.attnTs[k], A.h_attnTs[k]
        hc = A.h_const
        scale = 128 ** -0.5
        sc2 = A.sbscale2
        for i in range(c.NOWN):
            qsl = slice(i * 128, (i + 1) * 128)
            acc, hacc = A.acc.next()
            carry, hcar = A.carry.next()
            s.op("pool", lambda e: e.memset(acc[:], 0.0), writes=[hacc])
            s.op("pool", lambda e: e.memset(carry[:], 0.0), writes=[hcar])
            near = [(2 * i - 1 + j, j) for j in range(3) if 2 * i - 1 + j >= 0]
            groups = [("near", [kb for kb, j in near], near[0][1])]
            nfar = max(0, 2 * i - 1)
            hi_kb = nfar
            while hi_kb > 0:
                lo = max(0, hi_kb - 4)
                groups.append(("far", list(range(lo, hi_kb)), None))
                hi_kb = lo
            for kind, kbs, j0 in groups:
                n = len(kbs)
                W = n * 128
                bank, hb = A.sbank.next()
                for pos, kb in enumerate(kbs):
                    s.op("pe", lambda e: e.matmul(bank[:, pos * 128:(pos + 1) * 128], lhsT=kTs[:, kb * 128:(kb + 1) * 128], rhs=qTs[:, qsl], start=True, stop=True),
                         reads=[hi], writes=[hb], inc=(pos == n - 1), skip_self=True)
                eb, heb = A.ebuf.next()
                s.op("act", lambda e: e.activation(out=eb[:, 0:W], in_=bank[:, 0:W], func=AF.Exp, scale=scale), reads=[hb], writes=[heb])
                spb, hspb = A.spb.next()
                if kind == "far":
                    s.op("act", lambda e: e.activation(out=spb[:, 0:W], in_=eb[:, 0:W], func=AF.Ln, bias=1.0), reads=[heb], writes=[hspb])
                else:
                    spf, hspf = A.spf.next()
                    s.op("act", lambda e: e.activation(out=spf[:, 0:W], in_=eb[:, 0:W], func=AF.Ln, bias=1.0), reads=[heb], writes=[hspf])
                    s.op("dve", lambda e: e.tensor_tensor(out=spb[:, 0:W].rearrange("p (j q) -> p j q", j=n), in0=spf[:, 0:W].rearrange("p (j q) -> p j q", j=n),
                                                          in1=A.sbm01[:, j0:j0 + n, :], op=ALU.mult), reads=[hspf, hc], writes=[hspb])
                bank, hb = A.sbank.next()
                for pos, kb in enumerate(kbs):
                    s.op("pe", lambda e: e.matmul(bank[:, pos * 128:(pos + 1) * 128], lhsT=A.triN[:], rhs=spb[:, pos * 128:(pos + 1) * 128], start=True, stop=False),
                         reads=[hspb, hc], writes=[hb], inc=False, skip_self=True)
                    s.op("pe", lambda e: e.matmul(bank[:, pos * 128:(pos + 1) * 128], lhsT=kTs[:, kb * 128:(kb + 1) * 128], rhs=qTs[:, qsl], start=False, stop=True),
                         reads=[hi], writes=[hb], inc=(pos == n - 1), skip_self=True)
                A.sbg = getattr(A, "sbg", 0) + 1
                ob, hob = A.obank[0][A.sbg % 2]
                cb, hcb = A.obank[1][A.sbg % 2]
                for pos in range(n):
                    s.op("pe", lambda e: e.matmul(cb[:, pos:pos + 1], lhsT=spb[:, pos * 128:(pos + 1) * 128], rhs=A.onescol[:, 0:1], start=True, stop=True),
                         reads=[hspb, hc], writes=[hcb], inc=(pos == n - 1), skip_self=True)
                bt, hbt = A.et.next()
                if kind == "far":
                    s.op("act", lambda e: e.activation(out=bt[:, 0:W], in_=bank[:, 0:W], func=AF.Exp, scale=sc2), reads=[hb], writes=[hbt])
                else:
                    tf, htf = A.tmpf.next()
                    s.op("dve", lambda e: e.scalar_tensor_tensor(out=tf[:, 0:W].rearrange("p (j q) -> p j q", j=n), in0=bank[:, 0:W].rearrange("p (j q) -> p j q", j=n),
                                                                 scalar=sc2, in1=A.sbneg[:, j0:j0 + n, :], op0=ALU.mult, op1=ALU.add), reads=[hb, hc], writes=[htf])
                    s.op("act", lambda e: e.activation(out=bt[:, 0:W], in_=tf[:, 0:W], func=AF.Exp), reads=[htf], writes=[hbt])
                for pos, kb in enumerate(kbs):
                    s.op("pe", lambda e: e.matmul(ob[:, pos * 128:(pos + 1) * 128], lhsT=bt[:, pos * 128:(pos + 1) * 128], rhs=vaug[:, kb, 0:128], start=True, stop=True),
                         reads=[hbt, hi], writes=[hob], inc=(pos == n - 1), skip_self=True)
                sm, hsm = A.small.next()
                s.op("dve", lambda e: e.tensor_copy(out=sm[:, n - 1:n], in_=carry[:, 0:1]), reads=[hcar], writes=[hsm])
                for pos in range(n - 1, 0, -1):
                    s.op("dve", lambda e: e.tensor_tensor(out=sm[:, pos - 1:pos], in0=sm[:, pos:pos + 1], in1=cb[:, pos:pos + 1], op=ALU.add), reads=[hsm, hcb], writes=[hsm])
                s.op("dve", lambda e: e.tensor_tensor(out=carry[:, 0:1], in0=sm[:, 0:1], in1=cb[:, 0:1], op=ALU.add), reads=[hsm, hcb], writes=[hcar])
                s.op("act", lambda e: e.activation(out=sm[:, 8:8 + n], in_=sm[:, 0:n], func=AF.Exp, scale=-1.0), reads=[hsm], writes=[hsm])
                for pos in range(n):
                    s.op("dve", lambda e: e.scalar_tensor_tensor(out=acc[:], in0=ob[:, pos * 128:(pos + 1) * 128], scalar=sm[:, 8 + pos:9 + pos], in1=acc[:], op0=ALU.mult, op1=ALU.add),
                         reads=[hob, hsm, hacc], writes=[hacc])
            self.head_out(i, acc, hacc, attnTs, h_at, subnorm=False)
        s.dma("sp", t["attnT"][nh, :, :], attnTs[:], reads=[h_at])

    def bcast_load(self, dst, row_ap, h, width):
        self.s.dma("sp", dst, row_ap.to_broadcast([128, width]), writes=[h])

    def layer_norm_tile(self, hbuf, hh, xn, hxn, W):
        s, R = self.s, self.R
        D = W
        nchunk = (D + 511) // 512
        st_, hst = R.stats.next()
        for ci in range(nchunk):
            w0 = ci * 512
            w1 = min(D, w0 + 512)
            s.op("dve", lambda e: e.bn_stats(out=st_[:, ci * 6:(ci + 1) * 6], in_=hbuf[:, w0:w1]), reads=[hh], writes=[hst])
        s.op("dve", lambda e: e.bn_aggr(out=st_[:, 48:50], in_=st_[:, 0:nchunk * 6]), reads=[hst], writes=[hst])
        s.op("dve", lambda e: e.tensor_scalar(out=st_[:, 50:51], in0=st_[:, 49:50], scalar1=LN_EPS, scalar2=None, op0=ALU.add), reads=[hst], writes=[hst])
        s.op("act", lambda e: e.activation(out=st_[:, 51:52], in_=st_[:, 50:51], func=AF.Ln), reads=[hst], writes=[hst])
        s.op("act", lambda e: e.activation(out=st_[:, 52:53], in_=st_[:, 51:52], func=AF.Exp, scale=-0.5), reads=[hst], writes=[hst])
        s.op("dve", lambda e: e.scalar_tensor_tensor(out=st_[:, 53:54], in0=st_[:, 48:49], scalar=-1.0, in1=st_[:, 52:53], op0=ALU.mult, op1=ALU.mult), reads=[hst], writes=[hst])
        s.op("act", lambda e: e.activation(out=xn[:], in_=hbuf[:], func=AF.Identity, scale=st_[:, 52:53], bias=st_[:, 53:54]), reads=[hh, hst], writes=[hxn])

    def phase_E(self, l, li):
        nc, s, c, t = self.nc, self.s, self.cfg, self.t
        D, DC, TO, NH, E, CAP = c.D, c.DC, c.TO, c.NH, c.E, c.CAP
        NT = TO // 128
        NBK = D // 512 if D >= 512 else 1
        BW = min(512, D)
        with ExitStack() as st:
            sb = lambda name, shape, dt: st.enter_context(nc.sbuf_tensor(self.nm(name), shape, dt))
            R = type("R", (), {})()
            self.R = R
            wo = sb("wo", [128, NH, D], BF16); h_wo = H()
            g1b = sb("g1b", [128, D], F32); lng = sb("lng", [128, D], F32); lnb = sb("lnb", [128, D], F32)
            G2 = sb("G2", [128, D], F32); B2 = sb("B2", [128, D], F32); hcst = H()
            at = Ring([(sb("at%d" % i, [128, NH, 128], BF16), H()) for i in range(2)])
            xt = Ring([(sb("xt%d" % i, [128, D], F32), H()) for i in range(2)])
            hb_ = Ring([(sb("hbuf%d" % i, [128, D], F32), H()) for i in range(2)])
            xn_ = Ring([(sb("xn%d" % i, [128, D], F32), H()) for i in range(2)])
            u2b_ = Ring([(sb("u2b%d" % i, [128, D], BF16), H()) for i in range(2)])
            u2T = sb("u2T", [128, DC, 128], F32); h_u2T = H()
            wr = sb("wr", [128, DC, 4 + E], F32)
            bgr = sb("bgr", [128, 4 + E], F32)
            ebase = sb("ebase", [128, E], F32)
            stri = sb("stri", [128, 128], BF16); onesm = sb("onesm", [128, 128], BF16); strif = sb("strif", [128, 128], F32)
            cum = sb("cum", [128, E], F32); h_cum = H()
            zt = sb("zt", [128, D], BF16); h_zt = H()
            R.stats = Ring([(sb("stats%d" % i, [128, 64], F32), H()) for i in range(2)])
            rt_ = Ring([(sb("rt%d" % i, [128, 256], F32), H()) for i in range(2)])
            maskb_ = Ring([(sb("maskb%d" % i, [128, E], BF16), H()) for i in range(2)])
            mixb = [(st.enter_context(nc.psum_tensor(self.nm("psM%d" % i), [128, 512], F32)), H()) for i in range(NBK)]
            tpb = Ring([(st.enter_context(nc.psum_tensor(self.nm("psTr%d" % i), [128, 512], F32)), H()) for i in range(2)])
            lgb = (st.enter_context(nc.psum_tensor(self.nm("psLg"), [128, 512], F32)), H())
            posb = (st.enter_context(nc.psum_tensor(self.nm("psPos"), [128, 512], F32)), H())
            for ch in range(NH):
                s.dma("pool", wo[:, ch, :], t["w_o"][l, ch * 128:(ch + 1) * 128, :], writes=[h_wo])
            s.dma("sp", wr[:], t["w_gr"][l, :, :].rearrange("(kc p) n -> p kc n", p=128), writes=[hcst])
            self.bcast_load(bgr[:], t["b_gr"][l:l + 1, :], hcst, 4 + E)
            self.bcast_load(g1b[:], t["modd"][0:1, 2 * D:3 * D], hcst, D)
            self.bcast_load(lng[:], t["ln_g"][l:l + 1, 0:D], hcst, D)
            self.bcast_load(lnb[:], t["ln_b"][l:l + 1, 0:D], hcst, D)
            self.bcast_load(G2[:], t["modd"][0:1, 4 * D:5 * D], hcst, D)
            self.bcast_load(B2[:], t["modd"][0:1, 3 * D:4 * D], hcst, D)
            s.op("dve", lambda e: e.tensor_scalar(out=g1b[:], in0=g1b[:], scalar1=1.0, scalar2=None, op0=ALU.add), reads=[hcst], writes=[hcst])
            s.op("dve", lambda e: e.tensor_scalar(out=G2[:], in0=G2[:], scalar1=1.0, scalar2=None, op0=ALU.add), reads=[hcst], writes=[hcst])
            xt0, hxt0 = xt.items[0]
            s.op("dve", lambda e: e.tensor_tensor(out=xt0[:], in0=lnb[:], in1=G2[:], op=ALU.mult), reads=[hcst], writes=[hxt0])
            s.op("dve", lambda e: e.tensor_tensor(out=B2[:], in0=B2[:], in1=xt0[:], op=ALU.add), reads=[hcst, hxt0], writes=[hcst])
            s.op("dve", lambda e: e.tensor_tensor(out=G2[:], in0=G2[:], in1=lng[:], op=ALU.mult), reads=[hcst], writes=[hcst])
            s.op("pool", lambda e: e.iota(ebase[:], pattern=[[CAP, E]], base=0, channel_multiplier=0, allow_small_or_imprecise_dtypes=True), writes=[hcst])
            s.op("pool", lambda e: e.memset(strif[:], 1.0), reads=[hcst], writes=[hcst])
            s.op("pool", lambda e: e.affine_select(out=strif[:], in_=strif[:], pattern=[[1, 128]], compare_op=ALU.is_gt, fill=0.0, base=0, channel_multiplier=-1),
                 reads=[hcst], writes=[hcst])
            s.op("pool", lambda e: e.tensor_copy(out=stri[:], in_=strif[:]), reads=[hcst], writes=[hcst])
            s.op("pool", lambda e: e.memset(onesm[:], 1.0), writes=[hcst])
            s.op("pool", lambda e: e.memset(cum[:], 0.0), writes=[h_cum])
            s.op("pool", lambda e: e.memset(zt[:], 0.0), writes=[h_zt])
            h_xg = self.h_xg = H()
            for r0 in range(0, E * CAP, 128):
                s.dma("sp", t["xg"][r0:r0 + 128, :], zt[:], reads=[h_zt], writes=[h_xg])
            for tt in range(NT):
                tsl = slice(tt * 128, (tt + 1) * 128)
                a_t, h_at = at.next()
                s.dma("sp", a_t[:], t["attnT"][:, :, tsl].rearrange("h p s -> p h s"), writes=[h_at])
                x_t, h_xt = xt.next()
                s.dma("sp", x_t[:], self.src_own(li, tt * 128, 128)[0][2][:, 0, :], writes=[h_xt])
                for nb in range(NBK):
                    bank, hbk = mixb[nb]
                    for ch in range(NH):
                        s.op("pe", lambda e: e.matmul(bank[:, 0:BW], lhsT=a_t[:, ch, :], rhs=wo[:, ch, nb * BW:(nb + 1) * BW], start=(ch == 0), stop=(ch == NH - 1)),
                             reads=[h_at, h_wo], writes=[hbk], inc=(ch == NH - 1), skip_self=True)
                hbuf, hh = hb_.next()
                for nb in range(NBK):
                    bank, hbk = mixb[nb]
                    s.op("dve", lambda e: e.tensor_tensor(out=hbuf[:, nb * BW:(nb + 1) * BW], in0=bank[:, 0:BW], in1=g1b[:, nb * BW:(nb + 1) * BW], op=ALU.mult),
                         reads=[hbk, hcst], writes=[hh])
                s.op("dve", lambda e: e.scalar_tensor_tensor(out=hbuf[:], in0=x_t[:], scalar=float(c.alpha), in1=hbuf[:], op0=ALU.mult, op1=ALU.add),
                     reads=[h_xt, hh], writes=[hh])
                xn, hxn = xn_.next()
                self.layer_norm_tile(hbuf, hh, xn, hxn, D)
                s.op("dve", lambda e: e.tensor_tensor(out=hbuf[:], in0=xn[:], in1=lng[:], op=ALU.mult), reads=[hxn, hcst], writes=[hh])
                s.op("dve", lambda e: e.tensor_tensor(out=hbuf[:], in0=hbuf[:], in1=lnb[:], op=ALU.add), reads=[hh, hcst], writes=[hh])
                s.dma("sp", t["x1"][tsl, :], hbuf[:], reads=[hh])
                s.op("pool", lambda e: e.tensor_tensor(out=x_t[:], in0=xn[:], in1=G2[:], op=ALU.mult), reads=[hxn, hcst], writes=[h_xt])
                s.op("pool", lambda e: e.tensor_tensor(out=x_t[:], in0=x_t[:], in1=B2[:], op=ALU.add), reads=[h_xt, hcst], writes=[h_xt])
                u2b, hu2b = u2b_.next()
                s.op("act", lambda e: e.copy(out=u2b[:], in_=x_t[:]), reads=[h_xt], writes=[hu2b])
                for k0 in range(0, DC, 4):
                    kn = min(4, DC - k0)
                    bank, hbk = tpb.next()
                    for kk in range(kn):
                        kc = k0 + kk
                        s.op("pe", lambda e: e.transpose(bank[:, kk * 128:(kk + 1) * 128], x_t[:, kc * 128:(kc + 1) * 128], self.ident[:]),
                             reads=[h_xt, self.h_ident], writes=[hbk], inc=(kk == kn - 1), skip_self=True)
                    s.op("dve", lambda e: e.tensor_copy(out=u2T[:, k0:k0 + kn, :], in_=bank[:, 0:kn * 128].rearrange("p (k q) -> p k q", k=kn)), reads=[hbk], writes=[h_u2T])
                lg, hlg = lgb
                for kc in range(DC):
                    s.op("pe", lambda e: e.matmul(lg[:, 0:4 + E], lhsT=u2T[:, kc, :], rhs=wr[:, kc, :], start=(kc == 0), stop=(kc == DC - 1)),
                         reads=[h_u2T, hcst], writes=[hlg], inc=(kc == DC - 1), skip_self=True)
                self.route_tile(tt, lg, hlg, bgr, ebase, stri, onesm, cum, h_cum, hcst, rt_, maskb_, posb)
                for k in range(2):
                    s.dma("pool", None, None, reads=[hu2b, self.h_slots], writes=[h_xg], join=False,
                          indirect=lambda e: e.indirect_dma_start(out=t["xg"][:, :], out_offset=bass.IndirectOffsetOnAxis(ap=self.slots[:, tt, k:k + 1], axis=0),
                                                                  in_=u2b[:, :], in_offset=None, bounds_check=self.bounds_reg(), oob_is_err=False))
            if self.debug:
                s.dma("sp", t["dbg_route"].rearrange("(t p) k -> p t k", p=128)[:, :, 0:2], self.wts[:, :, :], reads=[self.h_wts])
                s.dma("sp", t["dbg_route"].rearrange("(t p) k -> p t k", p=128)[:, :, 2:4].bitcast(I32), self.slots[:, :, :], reads=[self.h_slots])

    def route_tile(self, tt, lg, hlg, bgr, ebase, stri, onesm, cum, h_cum, hcst, rt_, maskb_, posb):
        s, c = self.s, self.cfg
        E, CAP = c.E, c.CAP
        EPG = c.EPG
        rt, hrt = rt_.next()
        BIG = 1.0e9
        LG = slice(0, 4 + E); GL = slice(0, 4); EL = slice(4, 4 + E)
        o = 4 + E
        GMAX = slice(o, o + 1); NGMAX = slice(o + 1, o + 2); GSUM = slice(o + 2, o + 3); GP = slice(o + 3, o + 4)
        M1 = slice(o + 4, o + 5); M2 = slice(o + 5, o + 6); DD = slice(o + 6, o + 7); E2 = slice(o + 7, o + 8)
        W1 = slice(o + 8, o + 9); W2 = slice(o + 9, o + 10); S0 = slice(o + 10, o + 11); S1 = slice(o + 11, o + 12)
        o += 12
        OHG = slice(o, o + 4); PEN = slice(o + 4, o + 8); GEX = slice(o + 8, o + 12)
        o += 12
        MSEL = slice(o, o + E); o += E
        OH1 = slice(o, o + E); o += E
        OH2 = slice(o, o + E); o += E
        POS = slice(o, o + E); o += E
        TMP = slice(o, o + E); o += E
        assert o <= 256
        op = lambda fn, reads=(hrt,), writes=(hrt,): s.op("dve", fn, reads=list(reads), writes=list(writes))
        s.op("dve", lambda e: e.tensor_tensor(out=rt[:, LG], in0=lg[:, 0:4 + E], in1=bgr[:], op=ALU.add), reads=[hlg, hcst], writes=[hrt])
        op(lambda e: e.tensor_reduce(out=rt[:, GMAX], in_=rt[:, GL], axis=AX.X, op=ALU.max))
        op(lambda e: e.tensor_scalar(out=rt[:, OHG], in0=rt[:, GL], scalar1=rt[:, GMAX], scalar2=None, op0=ALU.is_ge))
        op(lambda e: e.tensor_scalar(out=rt[:, NGMAX], in0=rt[:, GMAX], scalar1=-1.0, scalar2=None, op0=ALU.mult))
        s.op("act", lambda e: e.activation(out=rt[:, GEX], in_=rt[:, GL], func=AF.Exp, bias=rt[:, NGMAX]), reads=[hrt], writes=[hrt])
        op(lambda e: e.tensor_reduce(out=rt[:, GSUM], in_=rt[:, GEX], axis=AX.X, op=ALU.add))
        op(lambda e: e.reciprocal(out=rt[:, GP], in_=rt[:, GSUM]))
        op(lambda e: e.tensor_scalar(out=rt[:, PEN], in0=rt[:, OHG], scalar1=-1.0, scalar2=BIG, op0=ALU.add, op1=ALU.mult))
        op(lambda e: e.tensor_tensor(out=rt[:, MSEL].rearrange("p (g k) -> p g k", g=c.G), in0=rt[:, EL].rearrange("p (g k) -> p g k", g=c.G),
                                     in1=rt[:, PEN].unsqueeze(2).to_broadcast([128, c.G, EPG]), op=ALU.add))
        op(lambda e: e.tensor_reduce(out=rt[:, M1], in_=rt[:, MSEL], axis=AX.X, op=ALU.max))
        op(lambda e: e.tensor_scalar(out=rt[:, OH1], in0=rt[:, MSEL], scalar1=rt[:, M1], scalar2=None, op0=ALU.is_ge))
        op(lambda e: e.scalar_tensor_tensor(out=rt[:, TMP], in0=rt[:, OH1], scalar=-BIG, in1=rt[:, MSEL], op0=ALU.mult, op1=ALU.add))
        op(lambda e: e.tensor_reduce(out=rt[:, M2], in_=rt[:, TMP], axis=AX.X, op=ALU.max))
        op(lambda e: e.tensor_scalar(out=rt[:, OH2], in0=rt[:, TMP], scalar1=rt[:, M2], scalar2=None, op0=ALU.is_ge))
        op(lambda e: e.tensor_tensor(out=rt[:, DD], in0=rt[:, M2], in1=rt[:, M1], op=ALU.subtract))
        s.op("act", lambda e: e.activation(out=rt[:, E2], in_=rt[:, DD], func=AF.Exp), reads=[hrt], writes=[hrt])
        op(lambda e: e.tensor_scalar(out=rt[:, W2], in0=rt[:, E2], scalar1=1.0, scalar2=None, op0=ALU.add))
        op(lambda e: e.reciprocal(out=rt[:, W1], in_=rt[:, W2]))
        s.op("dve", lambda e: e.tensor_tensor(out=self.wts[:, tt, 0:1], in0=rt[:, W1], in1=rt[:, GP], op=ALU.mult), reads=[hrt], writes=[self.h_wts])
        s.op("dve", lambda e: e.tensor_tensor(out=self.wts[:, tt, 1:2], in0=self.wts[:, tt, 0:1], in1=rt[:, E2], op=ALU.mult), reads=[hrt, self.h_wts], writes=[self.h_wts])
        mb, hmb = maskb_.next()
        s.op("dve", lambda e: e.tensor_tensor(out=mb[:], in0=rt[:, OH1], in1=rt[:, OH2], op=ALU.add), reads=[hrt], writes=[hmb])
        pb, hpb = posb
        s.op("pe", lambda e: e.matmul(pb[:, 0:E], lhsT=stri[:], rhs=mb[:], start=True, stop=True), reads=[hmb, hcst], writes=[hpb], inc=False, skip_self=True)
        s.op("pe", lambda e: e.matmul(pb[:, E:2 * E], lhsT=onesm[:], rhs=mb[:], start=True, stop=True), reads=[hmb, hcst], writes=[hpb], skip_self=True)
        s.op("dve", lambda e: e.tensor_tensor(out=rt[:, POS], in0=pb[:, 0:E], in1=cum[:], op=ALU.add), reads=[hpb, h_cum, hrt], writes=[hrt])
        s.op("dve", lambda e: e.tensor_tensor(out=cum[:], in0=pb[:, E:2 * E], in1=cum[:], op=ALU.add), reads=[hpb, h_cum], writes=[h_cum])
        op(lambda e: e.tensor_scalar(out=rt[:, TMP], in0=rt[:, POS], scalar1=float(CAP), scalar2=1.0e7, op0=ALU.is_ge, op1=ALU.mult))
        op(lambda e: e.tensor_tensor(out=rt[:, POS], in0=rt[:, POS], in1=rt[:, TMP], op=ALU.add))
        s.op("dve", lambda e: e.tensor_tensor(out=rt[:, POS], in0=rt[:, POS], in1=ebase[:], op=ALU.add), reads=[hrt, hcst], writes=[hrt])
        for k, OH, SS in ((0, OH1, S0), (1, OH2, S1)):
            op(lambda e: e.tensor_tensor(out=rt[:, TMP], in0=rt[:, POS], in1=rt[:, OH], op=ALU.mult))
            op(lambda e: e.tensor_reduce(out=rt[:, SS], in_=rt[:, TMP], axis=AX.X, op=ALU.add))
            s.op("dve", lambda e: e.tensor_copy(out=self.slots[:, tt, k:k + 1], in_=rt[:, SS]), reads=[hrt], writes=[self.h_slots])

    def phase_F(self, l):
        nc, s, c, t = self.nc, self.s, self.cfg, self.t
        D, DC, E, F, FC, CAP = c.D, c.DC, c.E, c.F, c.FC, c.CAP
        FW = min(512, F)
        FGN = F // FW
        FCP = min(4, FC)
        NDP = FC // FCP
        NST = CAP // 128
        BW = min(512, D)
        NBK = D // BW
        PIECE = max(DC * FW, FCP * D)
        with ExitStack() as st:
            sb = lambda name, shape, dt: st.enter_context(nc.sbuf_tensor(self.nm(name), shape, dt))
            wring = Ring([(sb("wring%d" % i, [128, PIECE], BF16), H()) for i in range(8)])
            xgs = Ring([(sb("xgs%d" % i, [128, NST, D], BF16), H()) for i in range(2)])
            xgT_ = Ring([(sb("xgT%d" % i, [128, DC, CAP], BF16), H()) for i in range(2)])
            hT_ = Ring([(sb("hT%d" % i, [128, FC, CAP], BF16), H()) for i in range(2)])
            sg_ = Ring([(sb("sg%d" % i, [128, CAP], F32), H()) for i in range(2)])
            yo_ = Ring([(sb("yo%d" % i, [128, D], F32), H()) for i in range(2)])
            gb = Ring([(st.enter_context(nc.psum_tensor(self.nm("psG%d" % i), [128, 512], F32)), H()) for i in range(2)])
            ub = Ring([(st.enter_context(nc.psum_tensor(self.nm("psU%d" % i), [128, 512], F32)), H()) for i in range(2)])
            yb = Ring([(st.enter_context(nc.psum_tensor(self.nm("psY%d" % i), [128, 512], F32)), H()) for i in range(2)])
            tpb = Ring([(st.enter_context(nc.psum_tensor(self.nm("psX%d" % i), [128, 1024], BF16)), H()) for i in range(2)])
            nev = 0
            for ex in range(E):
                gp, up, dp = [], [], []
                for fg in range(FGN):
                    for (lst, name) in ((gp, "w_gate"), (up, "w_up")):
                        w, hw = wring.next()
                        wv = w[:, 0:DC * FW].rearrange("p (kc f) -> p kc f", kc=DC)
                        s.dma("pool", wv, t[name][l, ex, :, fg * FW:(fg + 1) * FW].rearrange("(kc p) f -> p kc f", p=128), writes=[hw])
                        lst.append((wv, hw))
                for pi in range(NDP):
                    w, hw = wring.next()
                    wv = w[:, 0:FCP * D].rearrange("p (fc d) -> p fc d", fc=FCP)
                    s.dma("pool", wv, t["w_down"][l, ex, pi * FCP * 128:(pi + 1) * FCP * 128, :].rearrange("(fc p) d -> p fc d", p=128), writes=[hw])
                    dp.append((wv, hw))
                xs, hxs = xgs.next()
                s.dma("sp", xs[:], t["xg"][ex * CAP:(ex + 1) * CAP, :].rearrange("(st p) d -> p st d", p=128), writes=[hxs])
                xgT, hxT = xgT_.next()
                for st_i in range(NST):
                    for k0 in range(0, DC, 4):
                        kn = min(4, DC - k0)
                        bank, hbk = tpb.next()
                        for kk in range(kn):
                            kc = k0 + kk
                            s.op("pe", lambda e: e.transpose(bank[:, kk * 128:(kk + 1) * 128], xs[:, st_i, kc * 128:(kc + 1) * 128], self.identb[:]),
                                 reads=[hxs, self.h_identb], writes=[hbk], inc=(kk == kn - 1), skip_self=True)
                        self.evac(nev, xgT[:, k0:k0 + kn, st_i * 128:(st_i + 1) * 128], bank[:, 0:kn * 128].rearrange("p (k q) -> p k q", k=kn), hbk, hxT); nev += 1
                hT, hhT = hT_.next()
                for fg in range(FGN):
                    (wg, hwg), (wu, hwu) = gp[fg], up[fg]
                    for fl in range(FW // 128):
                        fc = fg * (FW // 128) + fl
                        g_, hg_ = gb.next()
                        u_, hu_ = ub.next()
                        for (bank, hbk, w, hw) in ((g_, hg_, wg, hwg), (u_, hu_, wu, hwu)):
                            for kc in range(DC):
                                s.op("pe", lambda e: e.matmul(bank[:, 0:CAP], lhsT=w[:, kc, fl * 128:(fl + 1) * 128], rhs=xgT[:, kc, :], start=(kc == 0), stop=(kc == DC - 1)),
                                     reads=[hw, hxT], writes=[hbk], inc=(kc == DC - 1), skip_self=True)
                        sg, hsg = sg_.next()
                        s.op("act", lambda e: e.activation(out=sg[:], in_=g_[:, 0:CAP], func=AF.Silu), reads=[hg_], writes=[hsg])
                        s.op("dve", lambda e: e.tensor_tensor(out=hT[:, fc, :], in0=u_[:, 0:CAP], in1=sg[:], op=ALU.mult), reads=[hu_, hsg], writes=[hhT])
                for st_i in range(NST):
                    yo, hyo = yo_.next()
                    for nb in range(NBK):
                        bank, hbk = yb.next()
                        for fc in range(FC):
                            wd, hwd = dp[fc // FCP]
                            s.op("pe", lambda e: e.matmul(bank[:, 0:BW], lhsT=hT[:, fc, st_i * 128:(st_i + 1) * 128], rhs=wd[:, fc % FCP, nb * BW:(nb + 1) * BW], start=(fc == 0), stop=(fc == FC - 1)),
                                 reads=[hhT, hwd], writes=[hbk], inc=(fc == FC - 1), skip_self=True)
                        self.evac(nev, yo[:, nb * BW:(nb + 1) * BW], bank[:, 0:BW], hbk, hyo); nev += 1
                    r0 = ex * CAP + st_i * 128
                    s.dma("sp", t["yg"][r0:r0 + 128, :], yo[:], reads=[hyo])

    def phase_G(self, l, li, last):
        nc, s, c, t = self.nc, self.s, self.cfg, self.t
        D, TO, E, CAP = c.D, c.TO, c.E, c.CAP
        NT = TO // 128
        with ExitStack() as st:
            sb = lambda name, shape, dt: st.enter_context(nc.sbuf_tensor(self.nm(name), shape, dt))
            R = type("R", (), {})()
            self.R = R
            g2b = sb("g2b", [128, D], F32); lng = sb("lng2", [128, D], F32); lnb = sb("lnb2", [128, D], F32); hcst = H()
            y0_ = Ring([(sb("y0_%d" % i, [128, D], F32), H()) for i in range(2)])
            y1_ = Ring([(sb("y1_%d" % i, [128, D], F32), H()) for i in range(2)])
            x1_ = Ring([(sb("x1_%d" % i, [128, D], F32), H()) for i in range(2)])
            R.stats = Ring([(sb("statsG%d" % i, [128, 64], F32), H()) for i in range(2)])
            self.bcast_load(g2b[:], t["modd"][0:1, 5 * D:6 * D], hcst, D)
            self.bcast_load(lng[:], t["ln_g"][l:l + 1, D:2 * D], hcst, D)
            self.bcast_load(lnb[:], t["ln_b"][l:l + 1, D:2 * D], hcst, D)
            s.op("dve", lambda e: e.tensor_scalar(out=g2b[:], in0=g2b[:], scalar1=1.0, scalar2=None, op0=ALU.add), reads=[hcst], writes=[hcst])
            dst = t["out"] if last else t["xnext"]
            for tt in range(NT):
                tsl = slice(tt * 128, (tt + 1) * 128)
                y0, hy0 = y0_.next()
                y1, hy1 = y1_.next()
                x1, hx1 = x1_.next()
                for (y, hy, k) in ((y0, hy0, 0), (y1, hy1, 1)):
                    s.op("pool", lambda e: e.memset(y[:], 0.0), writes=[hy])
                    s.dma("pool", None, None, reads=[self.h_slots], writes=[hy], join=False,
                          indirect=lambda e: e.indirect_dma_start(out=y[:, :], out_offset=None, in_=t["yg"][:, :],
                                                                  in_offset=bass.IndirectOffsetOnAxis(ap=self.slots[:, tt, k:k + 1], axis=0),
                                                                  bounds_check=self.bounds_reg(), oob_is_err=False))
                s.dma("sp", x1[:], t["x1"][tsl, :], writes=[hx1])
                s.op("dve", lambda e: e.tensor_scalar(out=y0[:], in0=y0[:], scalar1=self.wts[:, tt, 0:1], scalar2=None, op0=ALU.mult), reads=[hy0, self.h_wts], writes=[hy0])
                s.op("dve", lambda e: e.scalar_tensor_tensor(out=y0[:], in0=y1[:], scalar=self.wts[:, tt, 1:2], in1=y0[:], op0=ALU.mult, op1=ALU.add), reads=[hy0, hy1, self.h_wts], writes=[hy0])
                s.op("pool", lambda e: e.tensor_tensor(out=y0[:], in0=y0[:], in1=g2b[:], op=ALU.mult), reads=[hy0, hcst], writes=[hy0])
                s.op("dve", lambda e: e.scalar_tensor_tensor(out=y0[:], in0=x1[:], scalar=float(c.alpha), in1=y0[:], op0=ALU.mult, op1=ALU.add), reads=[hy0, hx1], writes=[hy0])
                self.layer_norm_tile(y0, hy0, y1, hy1, D)
                s.op("pool", lambda e: e.tensor_tensor(out=x1[:], in0=y1[:], in1=lng[:], op=ALU.mult), reads=[hy1, hcst], writes=[hx1])
                s.op("dve", lambda e: e.tensor_tensor(out=x1[:], in0=x1[:], in1=lnb[:], op=ALU.add), reads=[hx1, hcst], writes=[hx1])
                s.dma("sp", dst[tsl, :], x1[:], reads=[hx1])


def t5_bucket_np(rel, num_buckets=32, max_distance=128):
    n = np.maximum(rel, 0)
    max_exact = num_buckets // 2
    large = max_exact + (np.log(np.maximum(n, 1).astype(np.float32) / np.float32(max_exact))
                         / np.float32(math.log(max_distance / max_exact)) * np.float32(num_buckets - max_exact)).astype(np.int32)
    large = np.minimum(large, num_buckets - 1)
    return np.where(n < max_exact, n, large)


def host_consts(cfg, rel_bias, parity):
    c = cfg
    kk = np.arange(128)[:, None]
    qq = np.arange(128)[None, :]
    nearb = np.zeros((c.HD, 128, 3, 2, 128), np.float32)
    sbm01 = np.zeros((128, 3, 128), np.float32)
    sbneg = np.zeros((128, 3, 128), np.float32)
    for j in range(3):
        delta = parity + 1 - j
        rel = delta * 128 + qq - kk
        causal = rel >= 0
        strict = rel > 0
        bucket = t5_bucket_np(rel)
        for h in range(c.HD):
            for m in range(2):
                nearb[h, :, j, m, :] = np.where(causal, rel_bias[bucket, h, m], np.float32(NEG))
        sbm01[:, j, :] = strict.astype(np.float32)
        sbneg[:, j, :] = np.where(strict, np.float32(0.0), np.float32(NEG))
    farb = np.zeros((128, c.HD * 2), np.float32)
    for h in range(c.HD):
        for m in range(2):
            farb[:, 2 * h + m] = rel_bias[31, h, m]
    return dict(nearb=nearb.reshape(c.HD, 128, 3 * 2 * 128), farb=farb,
                sbm01=sbm01.reshape(128, 3 * 128), sbneg=sbneg.reshape(128, 3 * 128))


def own_rows(cfg, parity):
    idx = []
    for i in range(cfg.NOWN):
        g = 2 * i + parity
        idx.append(np.arange(g * 128, (g + 1) * 128))
    return np.concatenate(idx)


def host_inputs(cfg, inp, core, layers, x_cur=None):
    c = cfg
    l0, l1 = layers[0], layers[-1] + 1
    b, p = core // 2, core % 2
    x = inp["x"] if x_cur is None else x_cur
    rows = own_rows(c, p)
    m = {}
    m["x_full"] = np.ascontiguousarray(x[b])
    m["x_own"] = np.ascontiguousarray(x[b][rows])
    m["c_lay"] = np.ascontiguousarray(inp["c"][b].reshape(c.DC, 128).T)
    m["w_ada"] = inp["w_ada"][l0:l1]
    m["b_ada"] = inp["b_ada"][l0:l1]
    m["w_in"] = inp["w_in"][l0:l1]
    m["dlam"] = inp["diff_lambda"].reshape(c.depth, 256)[l0:l1]
    m["subg"] = inp["diff_subln_g"][l0:l1]
    m["w_o"] = inp["w_o"][l0:l1]
    m["ln_g"] = inp["ln_g"].reshape(c.depth, 2 * c.D)[l0:l1]
    m["ln_b"] = inp["ln_b"].reshape(c.depth, 2 * c.D)[l0:l1]
    m["w_gr"] = inp["_w_gr"][l0:l1]
    m["b_gr"] = inp["_b_gr"][l0:l1]
    m["w_gate"] = inp["w_gate"][l0:l1]
    m["w_up"] = inp["w_up"][l0:l1]
    m["w_down"] = inp["w_down"][l0:l1]
    m.update(inp["_consts"][p])
    return m


def host_common(cfg, inp):
    inp = dict(inp)
    inp["_w_gr"] = np.ascontiguousarray(np.concatenate([inp["w_group"], inp["w_router"]], axis=-1))
    inp["_b_gr"] = np.ascontiguousarray(np.concatenate([inp["b_group"], inp["b_router"]], axis=-1))
    inp["_consts"] = [host_consts(cfg, inp["rel_bias"], p) for p in range(2)]
    return inp


def kernel(**inputs):
    cfg = Cfg()
    inp = {k: np.asarray(v) for k, v in inputs.items()}
    common = host_common(cfg, inp)
    n_cores = 8
    x_cur = None
    for l in range(cfg.depth):
        prog = Prog(cfg, layers=[l])
        nc = prog.build()
        maps = [host_inputs(cfg, common, core, [l], x_cur) for core in range(n_cores)]
        res = run_bass_kernel_spmd(nc, maps, core_ids=list(range(n_cores)))
        x_new = np.empty((n_cores // 2, cfg.S, cfg.D), np.float32)
        for core in range(n_cores):
            x_new[core // 2][own_rows(cfg, core % 2)] = np.asarray(res.results[core]["out"])
        x_cur = x_new
    return x_cur
```
